# Optimizing a Trainium2 kernel written in Bass

```python
import math
import jax, jax.numpy as jnp
from jax import lax
import numpy as np


D_MODEL = 1024
BATCH = 4
SEQ = 8192
DEPTH = 4

GRID_W = 64
CTX_LEN = 256
N_MIXERS = 3
N_LAYERS_A = (DEPTH + 2) // 3
N_LAYERS_B = (DEPTH + 1) // 3
N_LAYERS_C = DEPTH // 3
EPS = 1e-6
ROPE_BASE = 10000.0
Q_BLOCK = 128
N_MOD = 6

MLA_HEADS = 16
MLA_Q_RANK = 256
MLA_KV_RANK = 256
MLA_NOPE = 64
MLA_ROPE = 32
MLA_V = 64

DIFF_HEAD_DIM = 64
DIFF_HEADS = D_MODEL // (2 * DIFF_HEAD_DIM)

GLA_HEADS = 4
GLA_DK = D_MODEL // 2
GLA_DV = D_MODEL
GLA_GATE_RANK = 16
GLA_GATE_NORM = 16.0
GLA_CHUNK = 64

MOE_GROUPS = 4
MOE_EXPERTS = 4
MOE_TOP_K = 2
MOE_D_EXPERT = 512

kernel_name = 'hybrid_mla_diff_gla_hmoe_prefix_dit'


def rms_norm(x, gain):
    xf = x.astype(jnp.float32)
    xf = xf * lax.rsqrt(jnp.mean(xf * xf, axis=-1, keepdims=True) + EPS)
    return xf.astype(x.dtype) * gain


def modulate(h, shift, scale):
    return h * (1 + scale) + shift


def split_heads(t, n_heads):
    b, n, _ = t.shape
    return t.reshape(b, n, n_heads, -1).transpose(0, 2, 1, 3)


def merge_heads(t):
    b, h, n, d = t.shape
    return t.transpose(0, 2, 1, 3).reshape(b, n, h * d)


def axial_rope_tables(n_rows, rot_dim, dtype):
    row = jnp.repeat(jnp.arange(n_rows, dtype=jnp.float32), GRID_W)
    col = jnp.tile(jnp.arange(GRID_W, dtype=jnp.float32), n_rows)
    n_freq = rot_dim // 4
    inv_freq = ROPE_BASE ** (-jnp.arange(n_freq, dtype=jnp.float32) / n_freq)
    ang = jnp.concatenate([row[:, None] * inv_freq, col[:, None] * inv_freq], axis=-1)
    return jnp.cos(ang).astype(dtype), jnp.sin(ang).astype(dtype)


def apply_rope(t, cos, sin):
    t1, t2 = jnp.split(t, 2, axis=-1)
    return jnp.concatenate([t1 * cos - t2 * sin, t1 * sin + t2 * cos], axis=-1)


def to_blocks(t):
    b, h, n = t.shape[:3]
    t = t.reshape(b, h, n // Q_BLOCK, Q_BLOCK, *t.shape[3:])
    return jnp.moveaxis(t, 2, 0)


def from_blocks(t):
    t = jnp.moveaxis(t, 0, 2)
    return t.reshape(t.shape[0], t.shape[1], -1, t.shape[-1])


def mla_mixer(hc, hl, cos, sin, w_dqkv, q_norm, w_uq, kv_norm, w_ukv, w_o, ctx_out):
    scale = (MLA_NOPE + MLA_ROPE) ** -0.5

    def down(h):
        return jnp.split(h @ w_dqkv, [MLA_Q_RANK, MLA_Q_RANK + MLA_KV_RANK], axis=-1)

    def queries(cq):
        q = split_heads(rms_norm(cq, q_norm) @ w_uq, MLA_HEADS)
        return jnp.split(q, [MLA_NOPE], axis=-1)

    def keys_values(ckv):
        kv = split_heads(rms_norm(ckv, kv_norm) @ w_ukv, MLA_HEADS)
        return jnp.split(kv, [MLA_NOPE], axis=-1)

    def attend(q_nope, q_pe, k_nope, k_pe, v):
        s = jnp.einsum('bhqd,bhkd->bhqk', q_nope, k_nope) + jnp.einsum('bhqr,bkr->bhqk', q_pe, k_pe)
        p = jax.nn.softmax(s.astype(jnp.float32) * scale, axis=-1).astype(v.dtype)
        return jnp.einsum('bhqk,bhkd->bhqd', p, v)

    cq_c, ckv_c, kpe_c = down(hc)
    cq_l, ckv_l, kpe_l = down(hl)
    kn_c, v_c = keys_values(ckv_c)
    kn_l, v_l = keys_values(ckv_l)
    qn_l, qpe_l = queries(cq_l)
    qpe_l = apply_rope(qpe_l, cos, sin)
    kpe_l = apply_rope(kpe_l, cos, sin)
    kn_all = jnp.concatenate([kn_c, kn_l], axis=2)
    kpe_all = jnp.concatenate([kpe_c, kpe_l], axis=1)
    v_all = jnp.concatenate([v_c, v_l], axis=2)
    o_l = from_blocks(lax.map(lambda qb: attend(qb[0], qb[1], kn_all, kpe_all, v_all),
                              (to_blocks(qn_l), to_blocks(qpe_l))))
    y_l = merge_heads(o_l) @ w_o
    if not ctx_out:
        return None, y_l
    qn_c, qpe_c = queries(cq_c)
    y_c = merge_heads(attend(qn_c, qpe_c, kn_c, kpe_c, v_c)) @ w_o
    return y_c, y_l


def diff_mixer(hc, hl, cos, sin, w_qkv, lam, subln, w_o, lam_init, ctx_out):
    scale = DIFF_HEAD_DIM ** -0.5
    lam_full = (jnp.exp(jnp.sum(lam[0] * lam[1], dtype=jnp.float32))
                - jnp.exp(jnp.sum(lam[2] * lam[3], dtype=jnp.float32)) + lam_init)

    def project(h):
        b, n, _ = h.shape
        q, k, v = jnp.split(h @ w_qkv, 3, axis=-1)
        q = q.reshape(b, n, DIFF_HEADS, 2, DIFF_HEAD_DIM).transpose(0, 2, 1, 3, 4)
        k = k.reshape(b, n, DIFF_HEADS, 2, DIFF_HEAD_DIM).transpose(0, 2, 1, 3, 4)
        return q, k, split_heads(v, DIFF_HEADS)

    def attend(q, k, v):
        s = jnp.einsum('bhqsd,bhksd->bhsqk', q, k)
        p = jax.nn.softmax(s.astype(jnp.float32) * scale, axis=-1)
        a = (p[:, :, 0] - lam_full * p[:, :, 1]).astype(v.dtype)
        return jnp.einsum('bhqk,bhkd->bhqd', a, v)

    def out(o):
        return merge_heads(rms_norm(o, subln) * (1.0 - lam_init)) @ w_o

    q_c, k_c, v_c = project(hc)
    q_l, k_l, v_l = project(hl)
    rc, rs = cos[:, None, :], sin[:, None, :]
    q_l = apply_rope(q_l, rc, rs)
    k_l = apply_rope(k_l, rc, rs)
    k_all = jnp.concatenate([k_c, k_l], axis=2)
    v_all = jnp.concatenate([v_c, v_l], axis=2)
    o_l = from_blocks(lax.map(lambda qb: attend(qb, k_all, v_all), to_blocks(q_l)))
    y_l = out(o_l)
    if not ctx_out:
        return None, y_l
    return out(attend(q_c, k_c, v_c)), y_l


def gla_scan_chunked(q, k, v, log_a, s0, with_output):
    b, h, n, dk = q.shape
    dv = v.shape[-1]
    nc = n // GLA_CHUNK
    chunk = lambda t: t.reshape(b, h, nc, GLA_CHUNK, t.shape[-1])
    qc, kc, vc = chunk(q), chunk(k), chunk(v)
    cum = jnp.cumsum(chunk(log_a), axis=3)
    cum_last = cum[:, :, :, -1:, :]
    chunk_kv = jnp.einsum('bhcsk,bhcsv->bhckv', kc * jnp.exp(cum_last - cum), vc)
    decay = jnp.exp(cum_last[:, :, :, 0, :])

    def step(state, inp):
        dec, kv = inp
        return dec[..., None] * state + kv, (state if with_output else None)

    s_final, s_start = lax.scan(step, s0, (jnp.moveaxis(decay, 2, 0), jnp.moveaxis(chunk_kv, 2, 0)))
    if not with_output:
        return None, s_final
    s_start = jnp.moveaxis(s_start, 0, 2)
    q_dec = qc * jnp.exp(cum)
    k_inv = kc * jnp.exp(-cum)
    lower_tri = jnp.tril(jnp.ones((GLA_CHUNK, GLA_CHUNK), dtype=bool))
    att = jnp.where(lower_tri, jnp.einsum('bhcqk,bhcsk->bhcqs', q_dec, k_inv), 0.0)
    o = (jnp.einsum('bhcqk,bhckv->bhcqv', q_dec, s_start)
         + jnp.einsum('bhcqs,bhcsv->bhcqv', att, vc))
    return o.reshape(b, h, n, dv), s_final


def gla_mixer(hc, hl, w_in, w_gate_up, b_gate, head_norm, w_o, ctx_out):
    splits = [GLA_DK, 2 * GLA_DK, 2 * GLA_DK + GLA_DV, 2 * GLA_DK + 2 * GLA_DV,
              2 * GLA_DK + 2 * GLA_DV + GLA_GATE_RANK]
    dk_h, dv_h = GLA_DK // GLA_HEADS, GLA_DV // GLA_HEADS

    def log_decay(z, d):
        a = jax.nn.log_sigmoid((z @ w_gate_up[d] + b_gate[d]).astype(jnp.float32)) / GLA_GATE_NORM
        return split_heads(a, GLA_HEADS)

    def project(h):
        q, k, v, g, zf, zb = jnp.split(h @ w_in, splits, axis=-1)
        q = split_heads(q, GLA_HEADS) * dk_h ** -0.5
        return (q, split_heads(k, GLA_HEADS), split_heads(v, GLA_HEADS), g,
                log_decay(zf, 0), log_decay(zb, 1))

    flip = lambda t: jnp.flip(t, axis=2)

    def out(o, g):
        return (merge_heads(rms_norm(o.astype(g.dtype), head_norm)) * jax.nn.silu(g)) @ w_o

    q_c, k_c, v_c, g_c, af_c, ab_c = project(hc)
    q_l, k_l, v_l, g_l, af_l, ab_l = project(hl)
    zeros = jnp.zeros((hc.shape[0], GLA_HEADS, dk_h, dv_h), jnp.float32)
    o_cf, s_f = gla_scan_chunked(q_c, k_c, v_c, af_c, zeros, ctx_out)
    o_cb, s_b = gla_scan_chunked(flip(q_c), flip(k_c), flip(v_c), flip(ab_c), zeros, ctx_out)
    o_lf, _ = gla_scan_chunked(q_l, k_l, v_l, af_l, s_f, True)
    o_lb, _ = gla_scan_chunked(flip(q_l), flip(k_l), flip(v_l), flip(ab_l), s_b, True)
    y_l = out(o_lf + flip(o_lb), g_l)
    if not ctx_out:
        return None, y_l
    return out(o_cf + flip(o_cb), g_c), y_l


def hier_moe(h, w_group, b_group, w_router, b_router, w_gate, w_up, w_down):
    grp_logits = (h @ w_group + b_group).astype(jnp.float32)
    grp_prob = jax.nn.softmax(grp_logits, axis=-1)
    grp_onehot = jax.nn.one_hot(jnp.argmax(grp_logits, axis=-1), MOE_GROUPS, dtype=jnp.float32)
    grp_w = jnp.sum(grp_prob * grp_onehot, axis=-1, keepdims=True)
    exp_logits = jnp.einsum('bnd,gde->bnge', h, w_router) + b_router
    exp_logits = jnp.einsum('bnge,bng->bne', exp_logits.astype(jnp.float32), grp_onehot)
    top_val, top_idx = lax.top_k(exp_logits, MOE_TOP_K)
    top_w = jax.nn.softmax(top_val, axis=-1) * grp_w
    exp_gate = jnp.einsum('bnk,bnke->bne', top_w, jax.nn.one_hot(top_idx, MOE_EXPERTS, dtype=jnp.float32))
    gate = (grp_onehot[..., None] * exp_gate[..., None, :]).astype(h.dtype)
    y = jnp.zeros_like(h)
    for gi in range(MOE_GROUPS):
        a = jnp.einsum('bnd,edf->bnef', h, w_gate[gi])
        u = jnp.einsum('bnd,edf->bnef', h, w_up[gi])
        y = y + jnp.einsum('bnef,efd->bnd', jax.nn.silu(a) * u * gate[:, :, gi, :, None], w_down[gi])
    return y


def setup_inputs(seed: int = 0) -> dict:
    key = jax.random.key(seed)
    ks = iter(jax.random.split(key, 40))
    f32 = jnp.float32
    D = D_MODEL

    def w(shape, fan_in, mult=1.0):
        return jax.random.normal(next(ks), shape, f32) * (mult * fan_in ** -0.5)

    def gain(shape):
        return 1.0 + 0.05 * jax.random.normal(next(ks), shape, f32)

    def bias(shape, s=0.02):
        return s * jax.random.normal(next(ks), shape, f32)

    return {
        'x': jax.random.normal(next(ks), (BATCH, SEQ, D), f32),
        'c': jax.random.normal(next(ks), (BATCH, D), f32),
        'ctx': jax.random.normal(next(ks), (BATCH, CTX_LEN, D), f32),
        'c_ctx': jax.random.normal(next(ks), (D,), f32),
        'w_ada': w((DEPTH, D, N_MOD * D), D, 0.5),
        'b_ada': bias((DEPTH, N_MOD * D)),
        'norm_mix': gain((DEPTH, D)),
        'norm_ffn': gain((DEPTH, D)),
        'mla_w_dqkv': w((N_LAYERS_A, D, MLA_Q_RANK + MLA_KV_RANK + MLA_ROPE), D),
        'mla_q_norm': gain((N_LAYERS_A, MLA_Q_RANK)),
        'mla_w_uq': w((N_LAYERS_A, MLA_Q_RANK, MLA_HEADS * (MLA_NOPE + MLA_ROPE)), MLA_Q_RANK),
        'mla_kv_norm': gain((N_LAYERS_A, MLA_KV_RANK)),
        'mla_w_ukv': w((N_LAYERS_A, MLA_KV_RANK, MLA_HEADS * (MLA_NOPE + MLA_V)), MLA_KV_RANK),
        'mla_w_o': w((N_LAYERS_A, MLA_HEADS * MLA_V, D), MLA_HEADS * MLA_V),
        'diff_w_qkv': w((N_LAYERS_B, D, 3 * D), D),
        'diff_lambda': bias((N_LAYERS_B, 4, DIFF_HEAD_DIM), 0.1),
        'diff_subln': gain((N_LAYERS_B, 2 * DIFF_HEAD_DIM)),
        'diff_w_o': w((N_LAYERS_B, D, D), D),
        'gla_w_in': w((N_LAYERS_C, D, 2 * GLA_DK + 2 * GLA_DV + 2 * GLA_GATE_RANK), D),
        'gla_w_gate_up': w((N_LAYERS_C, 2, GLA_GATE_RANK, GLA_DK), GLA_GATE_RANK),
        'gla_b_gate': bias((N_LAYERS_C, 2, GLA_DK), 0.1),
        'gla_head_norm': gain((N_LAYERS_C, GLA_DV // GLA_HEADS)),
        'gla_w_o': w((N_LAYERS_C, GLA_DV, D), GLA_DV),
        'moe_w_group': w((DEPTH, D, MOE_GROUPS), D),
        'moe_b_group': bias((DEPTH, MOE_GROUPS), 0.01),
        'moe_w_router': w((DEPTH, MOE_GROUPS, D, MOE_EXPERTS), D),
        'moe_b_router': bias((DEPTH, MOE_GROUPS, MOE_EXPERTS), 0.01),
        'moe_w_gate': w((DEPTH, MOE_GROUPS, MOE_EXPERTS, D, MOE_D_EXPERT), D),
        'moe_w_up': w((DEPTH, MOE_GROUPS, MOE_EXPERTS, D, MOE_D_EXPERT), D),
        'moe_w_down': w((DEPTH, MOE_GROUPS, MOE_EXPERTS, MOE_D_EXPERT, D), MOE_D_EXPERT),
        'final_norm': gain((D,)),
    }


def reference(x, c, ctx, c_ctx, w_ada, b_ada, norm_mix, norm_ffn,
              mla_w_dqkv, mla_q_norm, mla_w_uq, mla_kv_norm, mla_w_ukv, mla_w_o,
              diff_w_qkv, diff_lambda, diff_subln, diff_w_o,
              gla_w_in, gla_w_gate_up, gla_b_gate, gla_head_norm, gla_w_o,
              moe_w_group, moe_b_group, moe_w_router, moe_b_router,
              moe_w_gate, moe_w_up, moe_w_down, final_norm):
    n_tok = x.shape[1]
    ROWS = n_tok // GRID_W
    cos_a, sin_a = axial_rope_tables(ROWS, MLA_ROPE, x.dtype)
    cos_b, sin_b = axial_rope_tables(ROWS, DIFF_HEAD_DIM, x.dtype)
    n_ctx = ctx.shape[1]
    xl, xc = x, ctx
    for li in range(DEPTH):
        last = li == DEPTH - 1
        mod_l = jnp.split((jax.nn.silu(c) @ w_ada[li] + b_ada[li])[:, None, :], N_MOD, axis=-1)
        mod_c = jnp.split(jax.nn.silu(c_ctx) @ w_ada[li] + b_ada[li], N_MOD, axis=-1)
        hl = modulate(rms_norm(xl, norm_mix[li]), mod_l[0], mod_l[1])
        hc = modulate(rms_norm(xc, norm_mix[li]), mod_c[0], mod_c[1])
        kind, j = li % N_MIXERS, li // N_MIXERS
        if kind == 0:
            yc, yl = mla_mixer(hc, hl, cos_a, sin_a, mla_w_dqkv[j], mla_q_norm[j], mla_w_uq[j],
                               mla_kv_norm[j], mla_w_ukv[j], mla_w_o[j], not last)
        elif kind == 1:
            lam_init = 0.8 - 0.6 * math.exp(-0.3 * li)
            yc, yl = diff_mixer(hc, hl, cos_b, sin_b, diff_w_qkv[j], diff_lambda[j], diff_subln[j],
                                diff_w_o[j], lam_init, not last)
        else:
            yc, yl = gla_mixer(hc, hl, gla_w_in[j], gla_w_gate_up[j], gla_b_gate[j],
                               gla_head_norm[j], gla_w_o[j], not last)
        xl = xl + mod_l[2] * yl
        moe = lambda h: hier_moe(h, moe_w_group[li], moe_b_group[li], moe_w_router[li], moe_b_router[li],
                                 moe_w_gate[li], moe_w_up[li], moe_w_down[li])
        hl = modulate(rms_norm(xl, norm_ffn[li]), mod_l[3], mod_l[4])
        if last:
            xl = xl + mod_l[5] * moe(hl)
        else:
            xc = xc + mod_c[2] * yc
            hc = modulate(rms_norm(xc, norm_ffn[li]), mod_c[3], mod_c[4])
            y = moe(jnp.concatenate([hc, hl], axis=1))
            xc = xc + mod_c[5] * y[:, :n_ctx]
            xl = xl + mod_l[5] * y[:, n_ctx:]
    return rms_norm(xl, final_norm)
```

```python
import math
import numpy as np
from contextlib import ExitStack
import concourse.bass as bass
import concourse.mybir as mybir
from concourse.bass_utils import run_bass_kernel_spmd
import ml_dtypes

F32 = mybir.dt.float32
BF16 = mybir.dt.bfloat16
I32 = mybir.dt.int32
AF = mybir.ActivationFunctionType
ALU = mybir.AluOpType
AX = mybir.AxisListType

ENGS = ['pe', 'act', 'dve', 'pool', 'sp']


class Buf:
    __slots__ = ('name', 'w', 'r', 'parent', 'excl')

    def __init__(self, name):
        self.name = name
        self.w = None
        self.r = {}
        self.parent = None
        self.excl = False


class DSem:
    __slots__ = ('idx', 'total', 'handle', 'kind')

    def __init__(self, idx):
        self.idx = idx
        self.total = 0
        self.handle = None


class V:
    __slots__ = ('ap', 'buf')

    def __init__(self, ap, buf):
        self.ap = ap
        self.buf = buf


class Tile:
    def __init__(self, t, name, dsem=None):
        self.t = t
        self.buf = Buf(name)
        self.dsem = dsem
        self.subs = {}

    def __getitem__(self, idx):
        return V(self.t[idx], self.buf)

    def subbuf(self, key):
        b = self.subs.get(key)
        if b is None:
            b = self.subs[key] = Buf(self.buf.name + str(key))
            b.parent = self.buf
        return b

    def sub(self, key, idx):
        b = self.subs.get(key)
        if b is None:
            b = self.subs[key] = Buf(self.buf.name + str(key))
            b.parent = self.buf
        return V(self.t[idx], b)


class Em:
    def __init__(self, nc, max_dsems=110):
        self.nc = nc
        self.streams = {e: [] for e in ENGS}
        self.seq = {e: 0 for e in ENGS}
        self.waited = {e: {} for e in ENGS}
        self.dsems = []
        self.max_dsems = max_dsems
        self.ctx = ExitStack()
        self.ninst = 0

    def sb(self, name, shape, dtype, ctx=None):
        t = (ctx or self.ctx).enter_context(self.nc.sbuf_tensor(name, list(shape), dtype))
        return Tile(t, name)

    def ps(self, name, shape, dtype, ctx=None):
        t = (ctx or self.ctx).enter_context(self.nc.psum_tensor(name, list(shape), dtype))
        return Tile(t, name)

    def dram(self, name, shape, dtype, kind="Internal"):
        t = self.nc.dram_tensor(name, list(shape), dtype, kind=kind)
        return Tile(t.ap(), name)

    def arena_init(self, ncols):
        self.arena = self.ctx.enter_context(self.nc.sbuf_tensor("arena", [128, ncols], F32))
        self.arena_cols = ncols
        self.arena_off = 0
        self.psall = self.ctx.enter_context(self.nc.psum_tensor("psall", [128, 4096], F32))
        self._psb = [Buf("psum%d" % i) for i in range(8)]
        for b in self._psb:
            b.excl = True

    def alloc(self, name, ncols, dtype=F32, shape=None):
        w = ncols if dtype == F32 else (ncols + 1) // 2
        w = (w + 7) // 8 * 8
        assert self.arena_off + w <= self.arena_cols, "arena overflow %s %d" % (name, self.arena_off + w)
        ap = self.arena[:, self.arena_off:self.arena_off + w]
        off0 = self.arena_off
        self.arena_off += w
        if dtype != F32:
            ap = ap.bitcast(dtype)[:, 0:ncols]
        else:
            ap = ap[:, 0:ncols]
        t = Tile(ap, name)
        if not hasattr(self, '_arena_bufs'):
            self._arena_bufs = []
        self._arena_bufs.append((off0, t.buf))
        return t

    def mark(self):
        return self.arena_off

    def release(self, m):
        self.barrier()
        self.arena_off = m
        keep = []
        if not hasattr(self, '_free_dsems'):
            self._free_dsems = {'hw': [], 'sw': []}
        for off, b in getattr(self, '_arena_bufs', []):
            if off >= m:
                d = getattr(self, '_bs', {}).pop(id(b), None)
                if d is not None:
                    self._free_dsems[d.kind].append(d)
            else:
                keep.append((off, b))
        self._arena_bufs = keep

    def psum(self, bank, dtype=F32):
        return self.psum_span(bank, 1, dtype)

    def psum_span(self, bank, nb, dtype=F32):
        ap = self.psall[:, bank * 512:(bank + nb) * 512]
        if dtype != F32:
            ap = ap.bitcast(dtype)
        t = Tile(ap, "psum%d" % bank)
        t.buf = self._psb[bank] if nb == 1 else tuple(self._psb[bank:bank + nb])
        return t

    def new_dsem(self, kind='hw'):
        fl = getattr(self, '_free_dsems', None)
        if fl and fl.get(kind):
            return fl[kind].pop()
        d = DSem(len(self.dsems))
        d.kind = kind
        self.dsems.append(d)
        assert len(self.dsems) <= self.max_dsems, "too many dma semaphores"
        return d

    def _resolve(self, eng, reads, writes):
        deps = {}

        def add(ev):
            k, v = ev
            if isinstance(k, DSem):
                v = k.total
            if deps.get(k, 0) < v:
                deps[k] = v
        for b in reads:
            if b.w is not None:
                add(b.w)
            if b.excl:
                for k, v in b.r.items():
                    if k != eng:
                        add((k, v))
        for b in writes:
            if b.w is not None:
                add(b.w)
            for k, v in b.r.items():
                add((k, v))
        waits = []
        wd = self.waited[eng]
        for k, v in deps.items():
            if k == 'pe' and eng == 'pe':
                continue
            if wd.get(k, 0) >= v:
                continue
            wd[k] = v
            waits.append((k, v))
        return waits

    def op(self, eng, fn, reads=(), writes=()):
        reads = self._flat(reads)
        writes = self._flat(writes)
        waits = self._resolve(eng, reads, writes)
        self.seq[eng] += 1
        ev = (eng, self.seq[eng])
        self.streams[eng].append((waits, fn, eng, 1))
        self.ninst += 1
        for b in reads:
            if b.r.get(eng, 0) < ev[1]:
                b.r[eng] = ev[1]
        for b in writes:
            b.w = ev
            b.r = {}
        return ev

    @staticmethod
    def _flat(xs):
        out = []
        for x in xs:
            if x is None:
                continue
            b = x.buf if isinstance(x, V) else x
            if isinstance(b, tuple):
                out.extend(b)
            else:
                out.append(b)
        return out

    def dma(self, q, out, in_, dsem=None, **kw):
        if dsem is None:
            dsem = getattr(out.buf, '_dsem', None) if hasattr(out.buf, '_dsem') else None
        if dsem is None:
            dsem = self._bufsem(out.buf, 'sw' if q == 'pool' else 'hw')
        assert dsem.kind == ('sw' if q == 'pool' else 'hw'), "buffer receives DMAs from both queue kinds"
        waits = self._resolve(q, [in_.buf], [out.buf])
        dsem.total += 16
        ev = (dsem, dsem.total)
        oap, iap = out.ap, in_.ap
        if getattr(self.nc, '_allow_non_contiguous_dma_reason', None):
            kw = dict(kw)
            kw['allow_slow_non_contiguous'] = True
        self.streams[q].append((waits, lambda e: e.dma_start(out=oap, in_=iap, **kw), dsem, 16))
        self.ninst += 1
        in_.buf.r[dsem] = dsem.total
        out.buf.w = ev
        out.buf.r = {}
        return ev

    def _bufsem(self, buf, kind='hw'):
        if not hasattr(self, '_bs'):
            self._bs = {}
        if buf.parent is not None:
            buf = buf.parent
        d = self._bs.get(id(buf))
        if d is None:
            d = self._bs[id(buf)] = self.new_dsem(kind)
        return d

    def share_sem(self, bufs):
        d = self.new_dsem()
        if not hasattr(self, '_bs'):
            self._bs = {}
        for b in bufs:
            self._bs[id(b)] = d
        return d

    def barrier(self):
        for e in ENGS:
            waits = []
            wd = self.waited[e]
            for e2 in ENGS:
                if e2 == 'sp' or e2 == e:
                    continue
                v = self.seq[e2]
                if v > 0 and wd.get(e2, 0) < v:
                    wd[e2] = v
                    waits.append((e2, v))
            for d in self.dsems:
                if d.total > 0 and wd.get(d, 0) < d.total:
                    wd[d] = d.total
                    waits.append((d, d.total))
            if waits:
                self.streams[e].append((waits, None, None, 0))

    def mm(self, out, lhsT, rhs, start=True, stop=True):
        o, l, r = out.ap, lhsT.ap, rhs.ap
        return self.op('pe', lambda e: e.matmul(o, l, r, start=start, stop=stop),
                       reads=[lhsT, rhs], writes=[out])

    def tr(self, out, in_, ident):
        o, i, d = out.ap, in_.ap, ident.ap
        return self.op('pe', lambda e: e.transpose(o, i, d), reads=[in_, ident], writes=[out])

    def act(self, out, in_, func, bias=None, scale=1.0, accum=None, eng='act'):
        o, i = out.ap, in_.ap
        rd = [in_]
        kw = {}
        if isinstance(bias, V):
            rd.append(bias)
            kw['bias'] = bias.ap
        elif bias is not None:
            kw['bias'] = bias
        if isinstance(scale, V):
            rd.append(scale)
            kw['scale'] = scale.ap
        else:
            kw['scale'] = scale
        wr = [out]
        if accum is not None:
            wr.append(accum)
            kw['accum_out'] = accum.ap
        return self.op(eng, lambda e: e.activation(o, i, func, **kw), reads=rd, writes=wr)

    def tt(self, eng, out, in0, in1, op):
        o, a, b = out.ap, in0.ap, in1.ap
        return self.op(eng, lambda e: e.tensor_tensor(o, a, b, op), reads=[in0, in1], writes=[out])

    def ts(self, eng, out, in0, s1, s2=None, op0=ALU.mult, op1=None, accum=None):
        o, a = out.ap, in0.ap
        rd = [in0]
        a1 = s1
        if isinstance(s1, V):
            rd.append(s1)
            a1 = s1.ap
        a2 = s2
        if isinstance(s2, V):
            rd.append(s2)
            a2 = s2.ap
        wr = [out]
        kw = {}
        if op1 is not None:
            kw['op1'] = op1
        if accum is not None:
            wr.append(accum)
            kw['accum_out'] = accum.ap
        return self.op(eng, lambda e: e.tensor_scalar(o, a, a1, a2, op0, **kw), reads=rd, writes=wr)

    def stt(self, eng, out, in0, scalar, in1, op0, op1):
        o, a, b = out.ap, in0.ap, in1.ap
        rd = [in0, in1]
        s = scalar
        if isinstance(scalar, V):
            rd.append(scalar)
            s = scalar.ap
        return self.op(eng, lambda e: e.scalar_tensor_tensor(o, a, s, b, op0, op1), reads=rd, writes=[out])

    def copy(self, eng, out, in_):
        o, i = out.ap, in_.ap
        if eng == 'act':
            return self.op(eng, lambda e: e.copy(o, i), reads=[in_], writes=[out])
        return self.op(eng, lambda e: e.tensor_copy(o, i), reads=[in_], writes=[out])

    def recip(self, out, in_):
        o, i = out.ap, in_.ap
        return self.op('dve', lambda e: e.reciprocal(o, i), reads=[in_], writes=[out])

    def memset(self, eng, out, val):
        o = out.ap
        return self.op(eng, lambda e: e.memset(o, val), writes=[out])

    def reduce(self, eng, out, in_, op, axis=AX.X):
        o, i = out.ap, in_.ap
        return self.op(eng, lambda e: e.tensor_reduce(o, i, axis, op), reads=[in_], writes=[out])

    def emit(self):
        nc = self.nc
        self.barrier()
        with ExitStack() as st:
            esem = {e: st.enter_context(nc.semaphore("s_" + e)) for e in ENGS if e != 'sp'}
            for d in self.dsems:
                d.handle = st.enter_context(nc.semaphore("d%d" % d.idx))
            block = st.enter_context(nc.Block())

            def semof(k):
                return k.handle if isinstance(k, DSem) else esem[k]

            def run(e, stream):
                for waits, fn, inc, amt in stream:
                    for k, v in waits:
                        e.wait_ge(semof(k), v)
                    if fn is not None:
                        ins = fn(e)
                        ins.then_inc(semof(inc), amt)

            @block.tensor
            def _(e):
                run(e, self.streams['pe'])

            @block.scalar
            def _(e):
                run(e, self.streams['act'])

            @block.vector
            def _(e):
                run(e, self.streams['dve'])

            @block.gpsimd
            def _(e):
                run(e, self.streams['pool'])

            @block.sync
            def _(e):
                run(e, self.streams['sp'])
        self.ctx.close()

D = 1024
EPS = 1e-6


class P:
    pass


def r3(tile, pat, **kw):
    t = Tile(tile.t.rearrange(pat, **kw), "r3")
    t.buf = tile.buf
    return t


def bc_last(v, n):
    ap = v.ap
    return V(ap.unsqueeze(len(ap.shape)).to_broadcast(list(ap.shape) + [n]), v.buf)


def bc_mid(v, n):
    ap = v.ap
    return V(ap.unsqueeze(1).to_broadcast([ap.shape[0], n] + list(ap.shape[1:])), v.buf)


def rsqrt_col(em, out, in_, scale, eps, tmp):
    em.ts('dve', tmp, in_, scale, eps, op0=ALU.mult, op1=ALU.add)
    em.act(tmp, tmp, AF.Ln)
    em.act(out, tmp, AF.Exp, scale=-0.5)


def setup_consts(em, p):
    p.identf = em.alloc("identf", 128)
    em.dma('sp', p.identf[:, :], p.d_ident[:, :])
    p.ident = em.alloc("ident", 128, BF16)
    em.copy('dve', p.ident[:, :], p.identf[:, :])
    p.sel2 = em.alloc("sel2", 256)
    em.dma('sp', p.sel2[0:2, :], p.d_sel2[:, :])
    p.ones = em.alloc("ones", 128)
    em.memset('dve', p.ones[:, :], 1.0)
    p.onesb = em.alloc("onesb", 128, BF16)
    em.memset('dve', p.onesb[:, :], 1.0)


def prologue(em, p, l):
    nc = em.nc
    m0 = em.mark()
    sT = em.alloc("sT", 16)
    with nc.allow_non_contiguous_dma(reason="tiny transposed load"):
        for r in range(2):
            em.dma('sp', V(sT.t.rearrange("p (c r) -> p c r", r=2)[:, :, r], sT.buf),
                   V(p.d_cc.t[r].rearrange("(c p) -> p c", p=128), p.d_cc.buf))
    em.act(sT[:, :], sT[:, :], AF.Silu)
    modr = em.alloc("modr", 6 * D)
    bada = em.alloc("bada", 6 * D)
    for r in range(2):
        em.dma('sp', bada[r:r + 1, :], V(p.d_b_ada.t[l:l + 1, :], p.d_b_ada.buf))
    wa = [em.alloc("wa%d" % i, 8 * 512) for i in range(2)]
    wv = p.d_w_ada.t[l].rearrange("(c p) n -> p c n", p=128)
    for nb in range(12):
        w = wa[nb % 2]
        w3 = r3(w, "p (c n) -> p c n", c=8)
        em.dma('sp', w3[:, :, :], V(wv[:, :, nb * 512:(nb + 1) * 512], p.d_w_ada.buf))
        ps = em.psum(nb % 2)
        for c in range(8):
            em.mm(ps[0:2, 0:512], sT[:, 2 * c:2 * c + 2], w3[:, c, :], start=(c == 0), stop=(c == 7))
        em.tt('dve', modr[0:2, nb * 512:(nb + 1) * 512], ps[0:2, 0:512], bada[0:2, nb * 512:(nb + 1) * 512], ALU.add)
    for name, q in (("G2", 2), ("G5", 5)):
        g = getattr(p, name)
        for r in range(2):
            for hh in range(2):
                ps = em.psum(2 + hh)
                em.mm(ps[:, 0:512], p.sel2[0:2, r * 128:(r + 1) * 128], modr[0:2, q * D + hh * 512:q * D + (hh + 1) * 512])
                em.copy('act', g[:, r * D + hh * 512:r * D + (hh + 1) * 512], ps[:, 0:512])
    pc = em.psum(4)
    qs = [0, 1, 3, 4]
    for qi, q in enumerate(qs):
        for c in range(8):
            j = qi * 8 + c
            em.mm(pc[:, 2 * j:2 * j + 2], modr[0:2, q * D + c * 128:q * D + (c + 1) * 128], p.identf[0:2, 0:2])
    cols = em.alloc("cols", 64)
    em.copy('dve', cols[:, :], pc[:, 0:64])
    c4 = cols.t.rearrange("p (q c r) -> p q r c", q=4, c=8, r=2)
    gm = em.alloc("gm", 16)
    with nc.allow_non_contiguous_dma(reason="tiny transposed load"):
        em.dma('sp', gm[:, 0:8], V(p.d_norm_mix.t[l].rearrange("(c p) -> p c", p=128), p.d_norm_mix.buf))
        em.dma('sp', gm[:, 8:16], V(p.d_norm_ffn.t[l].rearrange("(c p) -> p c", p=128), p.d_norm_ffn.buf))
    for r in range(2):
        em.stt('dve', p.A1[:, r * 8:(r + 1) * 8], V(c4[:, 1, r, :], cols.buf), 1.0, gm[:, 0:8], ALU.add, ALU.mult)
        em.copy('dve', p.B1[:, r * 8:(r + 1) * 8], V(c4[:, 0, r, :], cols.buf))
        em.stt('dve', p.A2[:, r * 8:(r + 1) * 8], V(c4[:, 3, r, :], cols.buf), 1.0, gm[:, 8:16], ALU.add, ALU.mult)
        em.copy('dve', p.B2[:, r * 8:(r + 1) * 8], V(c4[:, 2, r, :], cols.buf))
    em.release(m0)


def alloc_mod(em, p):
    p.G2 = em.alloc("G2", 2 * D)
    p.G5 = em.alloc("G5", 2 * D)
    p.A1 = em.alloc("A1", 16)
    p.B1 = em.alloc("B1", 16)
    p.A2 = em.alloc("A2", 16)
    p.B2 = em.alloc("B2", 16)


class NormScratch:
    def __init__(self, em, tag):
        self.sq = em.alloc(tag + "sq", D)
        self.st = em.alloc(tag + "st", 4)
        self.xn = em.alloc(tag + "xn", D, BF16)
        self.tmp = em.alloc(tag + "tmp", D)


def norm_tile(em, p, ns, xt, A, B, r, hT_out, psbank):
    em.act(ns.sq[:, :], xt, AF.Square, accum=ns.st[:, 0:1])
    rsqrt_col(em, ns.st[:, 1:2], ns.st[:, 0:1], 1.0 / D, EPS, ns.st[:, 2:3])
    em.ts('dve', ns.xn[:, :], xt, ns.st[:, 1:2], None, op0=ALU.mult)
    pb = em.psum(psbank, BF16)
    for c in range(8):
        em.tr(pb[:, c * 128:(c + 1) * 128], ns.xn[:, c * 128:(c + 1) * 128], p.ident[:, :])
    t3 = V(ns.tmp.t.rearrange("p (c n) -> p c n", c=8), ns.tmp.buf)
    pb3 = V(pb.t.rearrange("p (c n) -> p c n", c=8), pb.buf)
    em.tt('dve', t3, pb3, bc_last(A[:, r * 8:(r + 1) * 8], 128), ALU.mult)
    em.tt('pool', hT_out, t3, bc_last(B[:, r * 8:(r + 1) * 8], 128), ALU.add)


def load_x_tile(em, p, xt, t, src=None):
    src = src or p.d_xs
    em.dma('sp', xt[:, :], src.sub(t, (slice(t * 128, (t + 1) * 128), slice(None))))


def phase_moe(em, p, l, last):
    nc = em.nc
    NT = p.NT
    m0 = em.mark()
    tiles = list(range(p.NC if last else 0, NT))
    per = 14
    sts = [tiles[i:i + per] for i in range(0, len(tiles), per)]
    wr = em.alloc("wr", 8 * 20, BF16)
    wr3 = r3(wr, "p (c n) -> p c n", c=8)
    with nc.allow_non_contiguous_dma(reason="small router weights"):
        em.dma('pool', wr3[:, :, 0:4], V(p.d_moe_w_group.t[l].rearrange("(c p) g -> p c g", p=128), p.d_moe_w_group.buf))
        for g in range(4):
            em.dma('pool', wr3[:, :, 4 + 4 * g:8 + 4 * g],
                   V(p.d_moe_w_router.t[l, g].rearrange("(c p) e -> p c e", p=128), p.d_moe_w_router.buf))
    rb = em.alloc("rb", 20)
    em.dma('sp', rb[:, 0:4], V(p.d_moe_b_group.t[l].partition_broadcast(128), p.d_moe_b_group.buf))
    em.dma('sp', rb[:, 4:20], V(p.d_moe_b_router.t[l].rearrange("g e -> (g e)").partition_broadcast(128), p.d_moe_b_router.buf))
    ns = NormScratch(em, "mo")
    xts = [em.alloc("moxt%d" % i, D) for i in range(2)]
    rt = em.alloc("rt", 128)
    wg = [em.alloc("wg%d" % i, 8 * 512, BF16) for i in range(2)]
    wu = [em.alloc("wu%d" % i, 8 * 512, BF16) for i in range(2)]
    wd = [em.alloc("wd%d" % i, 4 * 1024, BF16) for i in range(2)]
    sil = [em.alloc("sil%d" % i, 512) for i in range(2)]
    m1 = em.mark()
    for st in sts:
        n = len(st)
        hT = em.alloc("hT", 8 * n * 128, BF16)
        hT3 = r3(hT, "p (c t) -> p c t", c=8)
        gates = em.alloc("gates", n * 16)
        yacc = em.alloc("yacc", n * D)
        hh = em.alloc("hh", 4 * 512, BF16)
        hh3 = r3(hh, "p (f t) -> p f t", f=4)
        for i, t in enumerate(st):
            xt = xts[i % 2]
            load_x_tile(em, p, xt, t)
            r = 1 if t < p.NC else 0
            norm_tile(em, p, ns, xt[:, :], p.A2, p.B2, r, hT3[:, :, i * 128:(i + 1) * 128], 0)
            pr = em.psum(1)
            for c in range(8):
                em.mm(pr[:, 0:20], hT3[:, c, i * 128:(i + 1) * 128], wr3[:, c, :], start=(c == 0), stop=(c == 7))
            lg = rt[:, 0:20]
            em.tt('dve', lg, pr[:, 0:20], rb[:, 0:20], ALU.add)
            gmax = rt[:, 20:21]
            em.reduce('dve', gmax, rt[:, 0:4], ALU.max)
            oh = rt[:, 24:28]
            em.ts('dve', oh, rt[:, 0:4], gmax, None, op0=ALU.is_ge)
            ngmax = rt[:, 21:22]
            em.ts('dve', ngmax, gmax, -1.0, None, op0=ALU.mult)
            em.act(rt[:, 28:32], rt[:, 0:4], AF.Exp, bias=ngmax, accum=rt[:, 22:23])
            grpw = rt[:, 23:24]
            em.recip(grpw, rt[:, 22:23])
            sel = rt[:, 32:36]
            em.ts('dve', sel, rt[:, 4:8], rt[:, 24:25], None, op0=ALU.mult)
            for g in range(1, 4):
                em.stt('dve', sel, rt[:, 4 + 4 * g:8 + 4 * g], rt[:, 24 + g:25 + g], sel, ALU.mult, ALU.add)
            mx1 = rt[:, 36:37]
            em.reduce('dve', mx1, sel, ALU.max)
            mk1 = rt[:, 40:44]
            em.ts('dve', mk1, sel, mx1, None, op0=ALU.is_ge)
            sel2 = rt[:, 44:48]
            em.stt('dve', sel2, mk1, -1e30, sel, ALU.mult, ALU.add)
            mx2 = rt[:, 37:38]
            em.reduce('dve', mx2, sel2, ALU.max)
            mk2 = rt[:, 48:52]
            em.ts('dve', mk2, sel2, mx2, None, op0=ALU.is_ge)
            dd = rt[:, 38:39]
            em.tt('dve', dd, mx2, mx1, ALU.subtract)
            w2 = rt[:, 39:40]
            em.act(w2, dd, AF.Exp)
            w1 = rt[:, 52:53]
            em.ts('dve', w1, w2, 1.0, None, op0=ALU.add)
            em.recip(w1, w1)
            em.tt('dve', w2, w2, w1, ALU.mult)
            em.tt('dve', w1, w1, grpw, ALU.mult)
            em.tt('dve', w2, w2, grpw, ALU.mult)
            eg = rt[:, 56:60]
            em.ts('dve', eg, mk1, w1, None, op0=ALU.mult)
            em.stt('dve', eg, mk2, w2, eg, ALU.mult, ALU.add)
            for g in range(4):
                em.ts('dve', gates[:, i * 16 + 4 * g:i * 16 + 4 * g + 4], eg, rt[:, 24 + g:25 + g], None, op0=ALU.mult)
        blocks = [list(range(b0, min(b0 + 4, n))) for b0 in range(0, n, 4)]
        for e in range(16):
            g, ei = divmod(e, 4)
            s = e % 2
            wg3 = r3(wg[s], "p (c n) -> p c n", c=8)
            wu3 = r3(wu[s], "p (c n) -> p c n", c=8)
            wd3 = r3(wd[s], "p (c n) -> p c n", c=4)
            em.dma('pool', wg3[:, :, :], V(p.d_moe_w_gate.t[l, g, ei].rearrange("(c p) n -> p c n", p=128), p.d_moe_w_gate.buf))
            em.dma('pool', wu3[:, :, :], V(p.d_moe_w_up.t[l, g, ei].rearrange("(c p) n -> p c n", p=128), p.d_moe_w_up.buf))
            em.dma('pool', wd3[:, :, :], V(p.d_moe_w_down.t[l, g, ei].rearrange("(c p) n -> p c n", p=128), p.d_moe_w_down.buf))
            for blk in blocks:
                nt = len(blk) * 128
                t0 = blk[0] * 128
                for fc in range(4):
                    pa = em.psum(2 + (fc % 2))
                    pu = em.psum(4 + (fc % 2))
                    for c in range(8):
                        em.mm(pa[:, 0:nt], wg3[:, c, fc * 128:(fc + 1) * 128], hT3[:, c, t0:t0 + nt], start=(c == 0), stop=(c == 7))
                    for c in range(8):
                        em.mm(pu[:, 0:nt], wu3[:, c, fc * 128:(fc + 1) * 128], hT3[:, c, t0:t0 + nt], start=(c == 0), stop=(c == 7))
                    sl = sil[fc % 2]
                    em.act(sl[:, 0:nt], pa[:, 0:nt], AF.Silu)
                    em.tt('dve', hh3[:, fc, 0:nt], sl[:, 0:nt], pu[:, 0:nt], ALU.mult)
                for j, i in enumerate(blk):
                    for dh in range(2):
                        py = em.psum(6 + dh)
                        for fc in range(4):
                            em.mm(py[:, 0:512], hh3[:, fc, j * 128:(j + 1) * 128], wd3[:, fc, dh * 512:(dh + 1) * 512],
                                  start=(fc == 0), stop=(fc == 3))
                        ya = yacc[:, i * D + dh * 512:i * D + (dh + 1) * 512]
                        gcol = gates[:, i * 16 + e:i * 16 + e + 1]
                        if e == 0:
                            em.ts('dve', ya, py[:, 0:512], gcol, None, op0=ALU.mult)
                        else:
                            em.stt('dve', ya, py[:, 0:512], gcol, ya, ALU.mult, ALU.add)
        for i, t in enumerate(st):
            xt = xts[i % 2]
            load_x_tile(em, p, xt, t)
            r = 1 if t < p.NC else 0
            ya = yacc[:, i * D:(i + 1) * D]
            em.tt('pool', ya, ya, p.G5[:, r * D:(r + 1) * D], ALU.mult)
            em.tt('dve', ya, ya, xt[:, :], ALU.add)
            em.dma('sp', p.d_xs.sub(t, (slice(t * 128, (t + 1) * 128), slice(None))), ya)
        em.release(m1)
    em.release(m0)


def phase_final(em, p):
    m0 = em.mark()
    fn = em.alloc("fn", D)
    em.dma('sp', fn[:, :], V(p.d_final_norm.t.partition_broadcast(128), p.d_final_norm.buf))
    xts = [em.alloc("fxt%d" % i, D) for i in range(2)]
    sq = em.alloc("fsq", D)
    st = em.alloc("fst", 4)
    for t in range(p.NC, p.NT):
        xt = xts[t % 2]
        load_x_tile(em, p, xt, t)
        em.act(sq[:, :], xt[:, :], AF.Square, accum=st[:, 0:1])
        rsqrt_col(em, st[:, 1:2], st[:, 0:1], 1.0 / D, EPS, st[:, 2:3])
        em.stt('dve', xt[:, :], xt[:, :], st[:, 1:2], fn[:, :], ALU.mult, ALU.mult)
        em.dma('sp', p.d_out.sub(t, (slice((t - p.NC) * 128, (t - p.NC + 1) * 128), slice(None))), xt[:, :])
    em.release(m0)


def xs_tile(p, t):
    return p.d_xs.sub(t, (slice(t * 128, (t + 1) * 128), slice(None)))


def rope_apply(em, out1, out2, t1, t2, cos, sin, tmpa, tmpb):
    em.tt('dve', tmpa, t1, cos, ALU.mult)
    em.tt('dve', tmpb, t2, sin, ALU.mult)
    em.tt('dve', out1, tmpa, tmpb, ALU.subtract)
    em.tt('dve', tmpa, t1, sin, ALU.mult)
    em.tt('dve', tmpb, t2, cos, ALU.mult)
    em.tt('dve', out2, tmpa, tmpb, ALU.add)


def attention_core(em, p, m, nheads, dk, dv1, scale, last, finish, zsep=False):
    NT, NC = p.NT, p.NC
    NTOK = NT * 128
    kts = [em.alloc("kts%d" % i, NTOK, BF16) for i in range(2)]
    vas = [em.alloc("vas%d" % i, NT * dv1, BF16) for i in range(2)]
    qtb = [em.alloc("qtb%d" % i, 512, BF16) for i in range(2)]
    pts = [em.alloc("pts%d" % i, 1024, BF16) for i in range(2)]
    qblocks = []
    if not last:
        qblocks.append((0, NC * 128, list(range(NC))))
    for q0 in range(NC * 128, NTOK, 512):
        qblocks.append((q0, min(512, NTOK - q0), list(range(NT))))
    cnt = 0
    pcnt = 0
    for h in range(nheads):
        kt = kts[h % 2]
        em.dma('sp', kt[0:dk, :], V(p.d_KT.t[h], p.d_KT.buf))
        vh = m.vmap(h)
        if m.vmap(h - 1) != vh or h == 0:
            va = vas[vh % 2]
            va3 = r3(va, "p (t e) -> p t e", e=dv1)
            em.dma('sp', va3[:, :, :], V(p.d_VA.t[vh].rearrange("(t p) e -> p t e", p=128), p.d_VA.buf))
        for (q0, nq, ktl) in qblocks:
            qb = qtb[cnt % 2]
            em.dma('sp', qb[0:dk, 0:nq], V(p.d_QT.t[h, :, q0:q0 + nq], p.d_QT.buf))
            pO = em.psum(4 + cnt % 2)
            pZ = em.psum(6) if zsep else None
            cnt += 1
            pairs = [ktl[i:i + 2] for i in range(0, len(ktl), 2)]
            for pi, pr in enumerate(pairs):
                sb = em.psum_span(2 * (pcnt % 2), 2)
                pt = pts[pcnt % 2]
                pcnt += 1
                s3 = r3(sb, "p (j n) -> p j n", j=2)
                pt3 = r3(pt, "p (j n) -> p j n", j=2)
                for j, k in enumerate(pr):
                    em.mm(s3[:, j, 0:nq], kt[0:dk, k * 128:(k + 1) * 128], qb[0:dk, 0:nq])
                npj = len(pr)
                em.act(pt3[:, 0:npj, 0:nq], s3[:, 0:npj, 0:nq], AF.Exp, scale=scale)
                for j, k in enumerate(pr):
                    first = (pi == 0 and j == 0)
                    lastk = (pi == len(pairs) - 1 and j == npj - 1)
                    em.mm(pO[0:dv1, 0:nq], va3[:, k, :], pt3[:, j, 0:nq], start=first, stop=lastk)
                    if zsep:
                        em.mm(pZ[0:1, 0:nq], p.onesb[:, 0:1], pt3[:, j, 0:nq], start=first, stop=lastk)
            finish(h, q0, nq, pO, pZ)


def phase_mla(em, p, l, j, last):
    nc = em.nc
    NT, NC = p.NT, p.NC
    NTOK = NT * 128
    m0 = em.mark()
    wdq = em.alloc("wdq", 8 * 544, BF16)
    wdq3 = r3(wdq, "p (c n) -> p c n", c=8)
    em.dma('pool', wdq3[:, :, :], V(p.d_mla_w_dqkv.t[j].rearrange("(c p) n -> p c n", p=128), p.d_mla_w_dqkv.buf))
    wuq = em.alloc("wuq", 2 * 1536, BF16)
    wukv = em.alloc("wukv", 2 * 2048, BF16)
    gq = em.alloc("gq", 4)
    with nc.allow_non_contiguous_dma(reason="tiny"):
        em.dma('sp', gq[:, 0:2], V(p.d_mla_q_norm.t[j].rearrange("(c p) -> p c", p=128), p.d_mla_q_norm.buf))
        em.dma('sp', gq[:, 2:4], V(p.d_mla_kv_norm.t[j].rearrange("(c p) -> p c", p=128), p.d_mla_kv_norm.buf))
    m1 = em.mark()
    wtmp = em.alloc("wtmp", 2048)
    for (wsrc, wdst, width, a, b, goff) in ((p.d_mla_w_uq, wuq, 1536, 64, 32, 0), (p.d_mla_w_ukv, wukv, 2048, 64, 64, 2)):
        hd = a + b
        for c in range(2):
            src = wsrc.t[j, c * 128:(c + 1) * 128, :].rearrange("p (h e) -> p h e", e=hd)
            em.dma('sp', V(wtmp.t[:, 0:16 * a].rearrange("p (h e) -> p h e", e=a), wtmp.buf), V(src[:, :, 0:a], wsrc.buf))
            em.dma('sp', V(wtmp.t[:, 16 * a:16 * hd].rearrange("p (h e) -> p h e", e=b), wtmp.buf), V(src[:, :, a:hd], wsrc.buf))
            em.ts('dve', wdst[:, c * width:(c + 1) * width], wtmp[:, 0:width], gq[:, goff + c:goff + c + 1], None, op0=ALU.mult)
    em.release(m1)
    wuq3 = r3(wuq, "p (c n) -> p c n", c=2)
    wukv3 = r3(wukv, "p (c n) -> p c n", c=2)
    m1 = em.mark()
    ns = NormScratch(em, "ml")
    xts = [em.alloc("mlxt%d" % i, D) for i in range(2)]
    ropes = [em.alloc("mlrope%d" % i, 32) for i in range(2)]
    hT = em.alloc("mlhT", D, BF16)
    hT3 = r3(hT, "p (c t) -> p c t", c=8)
    st = em.alloc("mlst", 8)
    cn = em.alloc("mlcn", 512, BF16)
    cT = em.alloc("mlcT", 512, BF16)
    cT3 = r3(cT, "p (c t) -> p c t", c=4)
    kper = em.alloc("mlkpe", 32, BF16)
    tmpa = em.alloc("mltmpa", 512)
    tmpb = em.alloc("mltmpb", 512)
    sq = em.alloc("mlsq", 256)
    qs = em.alloc("mlqs", 16 * 96, BF16)
    ks = em.alloc("mlks", 16 * 96, BF16)
    qs3 = r3(qs, "p (h e) -> p h e", h=16)
    ks3 = r3(ks, "p (h e) -> p h e", h=16)
    vas = [em.alloc("mlva%d" % i, 16 * 65, BF16) for i in range(2)]
    for v in vas:
        em.memset('dve', v[:, :], 1.0)
    qTs = [em.alloc("mlqT%d" % i, 16 * 128, BF16) for i in range(2)]
    kTs = [em.alloc("mlkT%d" % i, 16 * 128, BF16) for i in range(2)]
    QTd = p.d_QT.t.rearrange("h r t -> r h t")
    KTd = p.d_KT.t.rearrange("h r t -> r h t")
    for t in range(NT):
        xt = xts[t % 2]
        rp = ropes[t % 2]
        load_x_tile(em, p, xt, t)
        em.dma('sp', rp[:, :], V(p.d_ropeA.t[t * 128:(t + 1) * 128, :], p.d_ropeA.buf))
        r = 1 if t < NC else 0
        norm_tile(em, p, ns, xt[:, :], p.A1, p.B1, r, hT3[:, :, :], 2)
        pd = em.psum(0)
        pk = em.psum(1)
        for c in range(8):
            em.mm(pd[:, 0:512], hT3[:, c, :], wdq3[:, c, 0:512], start=(c == 0), stop=(c == 7))
        for c in range(8):
            em.mm(pk[:, 0:32], hT3[:, c, :], wdq3[:, c, 512:544], start=(c == 0), stop=(c == 7))
        em.act(sq[:, :], pd[:, 0:256], AF.Square, accum=st[:, 0:1])
        em.act(sq[:, :], pd[:, 256:512], AF.Square, accum=st[:, 1:2])
        rsqrt_col(em, st[:, 2:4], st[:, 0:2], 1.0 / 256, EPS, st[:, 4:6])
        em.ts('dve', cn[:, 0:256], pd[:, 0:256], st[:, 2:3], None, op0=ALU.mult)
        em.ts('dve', cn[:, 256:512], pd[:, 256:512], st[:, 3:4], None, op0=ALU.mult)
        cos, sin = rp[:, 0:16], rp[:, 16:32]
        rope_apply(em, kper[:, 0:16], kper[:, 16:32], pk[:, 0:16], pk[:, 16:32], cos, sin, tmpa[:, 0:16], tmpb[:, 0:16])
        pt = em.psum(2, BF16)
        for c in range(4):
            em.tr(pt[:, c * 128:(c + 1) * 128], cn[:, c * 128:(c + 1) * 128], p.ident[:, :])
        em.copy('act', cT[:, :], pt[:, 0:512])
        pq = [em.psum(3), em.psum(4), em.psum(5)]
        for nb in range(3):
            for c in range(2):
                em.mm(pq[nb][:, 0:512], cT3[:, c, :], wuq3[:, c, nb * 512:(nb + 1) * 512], start=(c == 0), stop=(c == 1))
        for nb in range(2):
            em.copy('act', qs3[:, nb * 8:(nb + 1) * 8, 0:64], V(pq[nb].t.rearrange("p (h e) -> p h e", e=64), pq[nb].buf))
        qpe = pq[2].t[:, 0:512].rearrange("p (h e) -> p h e", e=32)
        ta = V(tmpa.t[:, 0:256].rearrange("p (h e) -> p h e", e=16), tmpa.buf)
        tb = V(tmpb.t[:, 0:256].rearrange("p (h e) -> p h e", e=16), tmpb.buf)
        rope_apply(em, qs3[:, :, 64:80], qs3[:, :, 80:96], V(qpe[:, :, 0:16], pq[2].buf), V(qpe[:, :, 16:32], pq[2].buf),
                   bc_mid(cos, 16), bc_mid(sin, 16), ta, tb)
        pkv = [em.psum(6), em.psum(7), em.psum(0), em.psum(1)]
        for nb in range(4):
            for c in range(2):
                em.mm(pkv[nb][:, 0:512], cT3[:, 2 + c, :], wukv3[:, c, nb * 512:(nb + 1) * 512], start=(c == 0), stop=(c == 1))
        va = vas[t % 2]
        va3 = r3(va, "p (h e) -> p h e", h=16)
        for nb in range(2):
            em.copy('act', ks3[:, nb * 8:(nb + 1) * 8, 0:64], V(pkv[nb].t.rearrange("p (h e) -> p h e", e=64), pkv[nb].buf))
            em.copy('dve', va3[:, nb * 8:(nb + 1) * 8, 0:64], V(pkv[2 + nb].t.rearrange("p (h e) -> p h e", e=64), pkv[2 + nb].buf))
        em.copy('pool', ks3[:, :, 64:96], bc_mid(kper[:, :], 16))
        em.dma('sp', V(p.d_VA.t.rearrange("h t e -> t h e")[t * 128:(t + 1) * 128, :, :], p.d_VA.subbuf(t)), va3[:, :, :])
        qT = qTs[t % 2]
        kT = kTs[t % 2]
        for (src3, dstT, dd) in ((qs3, qT, QTd), (ks3, kT, KTd)):
            for hb in range(2):
                ptr = em.psum(2 if hb == 0 else 3, BF16)
                for hh in range(8):
                    em.tr(ptr[0:96, hh * 128:(hh + 1) * 128], src3[:, hb * 8 + hh, :], p.ident[:, :])
                em.copy('act' if hb == 0 else 'dve', dstT[0:96, hb * 1024:(hb + 1) * 1024], ptr[0:96, :])
            em.dma('sp', V(dd[:, :, t * 128:(t + 1) * 128], p.d_QT.subbuf(t) if dd is QTd else p.d_KT.subbuf(t)),
                   V(dstT.t[0:96, :].rearrange("p (h t) -> p h t", h=16), dstT.buf))
    em.release(m1)
    m1 = em.mark()
    osb = [em.alloc("mlosb%d" % i, 512) for i in range(2)]
    rz = em.alloc("mlrz", 512)
    otn = [em.alloc("mlotn%d" % i, 512, BF16) for i in range(2)]
    fc = [0]

    def finish(h, q0, nq, pO, pZ):
        i = fc[0] % 2
        fc[0] += 1
        o = osb[i]
        em.copy('act', o[0:65, 0:nq], pO[0:65, 0:nq])
        em.recip(rz[64:65, 0:nq], o[64:65, 0:nq])
        pb = em.psum(6)
        em.mm(pb[0:64, 0:nq], p.ones[64:65, 0:64], rz[64:65, 0:nq])
        on = otn[i]
        em.tt('dve', on[0:64, 0:nq], o[0:64, 0:nq], pb[0:64, 0:nq], ALU.mult)
        em.dma('sp', V(p.d_OT.t[h, :, q0:q0 + nq], p.d_OT.subbuf((h, q0))), on[0:64, 0:nq])

    class M:
        @staticmethod
        def vmap(h):
            return h
    attention_core(em, p, M, 16, 96, 65, 96 ** -0.5, last, finish)
    em.release(m1)
    phase_oproj(em, p, p.d_mla_w_o, j, 16, 64, last)
    em.release(m0)


def phase_oproj(em, p, d_wo, j, nh, kd, last):
    NT, NC = p.NT, p.NC
    m1 = em.mark()
    wo = em.alloc("wo", nh * 1024, BF16)
    wo3 = r3(wo, "p (h n) -> p h n", h=nh)
    em.dma('pool', wo3[0:kd, :, :], V(d_wo.t[j].rearrange("(h p) n -> p h n", p=kd), d_wo.buf))
    ots = [em.alloc("ot%d" % i, nh * 128, BF16) for i in range(2)]
    xts = [em.alloc("xo%d" % i, D) for i in range(2)]
    ys = [em.alloc("ys%d" % i, D) for i in range(2)]
    OTd = p.d_OT.t.rearrange("h r t -> r h t")
    for t in range(NC if last else 0, NT):
        ot = ots[t % 2]
        ot3 = r3(ot, "p (h t) -> p h t", h=nh)
        em.dma('sp', ot3[0:kd, :, :], V(OTd[:, :, t * 128:(t + 1) * 128], p.d_OT.buf))
        xt = xts[t % 2]
        load_x_tile(em, p, xt, t)
        r = 1 if t < NC else 0
        y = ys[t % 2]
        for dh in range(2):
            py = em.psum(dh)
            for h in range(nh):
                em.mm(py[:, 0:512], ot3[0:kd, h, :], wo3[0:kd, h, dh * 512:(dh + 1) * 512], start=(h == 0), stop=(h == nh - 1))
            em.tt('dve', y[:, dh * 512:(dh + 1) * 512], py[:, 0:512], p.G2[:, r * D + dh * 512:r * D + (dh + 1) * 512], ALU.mult)
        em.tt('pool', y[:, :], y[:, :], xt[:, :], ALU.add)
        em.dma('sp', xs_tile(p, t), y[:, :])
    em.release(m1)


def phase_diff(em, p, l, j, last):
    nc = em.nc
    NT, NC = p.NT, p.NC
    NTOK = NT * 128
    lam_init = 0.8 - 0.6 * math.exp(-0.3 * l)
    m0 = em.mark()
    lamt = em.alloc("dflam", 256)
    em.dma('sp', lamt[:, :], V(p.d_diff_lambda.t[j].rearrange("a b -> (a b)").partition_broadcast(128), p.d_diff_lambda.buf))
    lsc = em.alloc("dflsc", 8)
    ltmp = em.alloc("dfltmp", 64)
    em.tt('dve', ltmp[:, :], lamt[:, 0:64], lamt[:, 64:128], ALU.mult)
    em.reduce('dve', lsc[:, 0:1], ltmp[:, :], ALU.add)
    em.tt('dve', ltmp[:, :], lamt[:, 128:192], lamt[:, 192:256], ALU.mult)
    em.reduce('dve', lsc[:, 1:2], ltmp[:, :], ALU.add)
    em.act(lsc[:, 2:4], lsc[:, 0:2], AF.Exp)
    em.tt('dve', lsc[:, 4:5], lsc[:, 3:4], lsc[:, 2:3], ALU.subtract)
    em.ts('dve', lsc[:, 5:6], lsc[:, 4:5], -lam_init, None, op0=ALU.add)
    nlam = lsc[:, 5:6]
    subc = em.alloc("dfsub", 2)
    with nc.allow_non_contiguous_dma(reason="tiny"):
        em.dma('sp', subc[:, 0:1], V(p.d_diff_subln.t[j].rearrange("(p o) -> p o", o=1), p.d_diff_subln.buf))
    em.ts('dve', subc[:, 1:2], subc[:, 0:1], 1.0 - lam_init, None, op0=ALU.mult)
    wq = em.alloc("dfwq", 8 * 3072, BF16)
    wq3 = r3(wq, "p (c n) -> p c n", c=8)
    for i in range(3):
        em.dma('pool', wq3[:, :, i * 1024:(i + 1) * 1024],
               V(p.d_diff_w_qkv.t[j].rearrange("(c p) n -> p c n", p=128)[:, :, i * 1024:(i + 1) * 1024], p.d_diff_w_qkv.buf))
    m1 = em.mark()
    ns = NormScratch(em, "df")
    xts = [em.alloc("dfxt%d" % i, D) for i in range(2)]
    ropes = [em.alloc("dfrope%d" % i, 64) for i in range(2)]
    hT = em.alloc("dfhT", D, BF16)
    hT3 = r3(hT, "p (c t) -> p c t", c=8)
    tmpa = em.alloc("dftmpa", 256)
    tmpb = em.alloc("dftmpb", 256)
    qs = em.alloc("dfqs", 1024, BF16)
    ks = em.alloc("dfks", 1024, BF16)
    qs3 = r3(qs, "p (h e) -> p h e", h=16)
    ks3 = r3(ks, "p (h e) -> p h e", h=16)
    vs = [em.alloc("dfvs%d" % i, 1024, BF16) for i in range(2)]
    qTs = [em.alloc("dfqT%d" % i, 16 * 128, BF16) for i in range(2)]
    kTs = [em.alloc("dfkT%d" % i, 16 * 128, BF16) for i in range(2)]
    QTd = p.d_QT.t.rearrange("h r t -> r h t")
    KTd = p.d_KT.t.rearrange("h r t -> r h t")
    ta = V(tmpa.t.rearrange("p (h e) -> p h e", e=32), tmpa.buf)
    tb = V(tmpb.t.rearrange("p (h e) -> p h e", e=32), tmpb.buf)
    for t in range(NT):
        xt = xts[t % 2]
        rp = ropes[t % 2]
        load_x_tile(em, p, xt, t)
        em.dma('sp', rp[:, :], V(p.d_ropeB.t[t * 128:(t + 1) * 128, :], p.d_ropeB.buf))
        r = 1 if t < NC else 0
        norm_tile(em, p, ns, xt[:, :], p.A1, p.B1, r, hT3[:, :, :], 6)
        cos, sin = rp[:, 0:32], rp[:, 32:64]
        pbs = [em.psum(i) for i in range(6)]
        for nb in range(6):
            for c in range(8):
                em.mm(pbs[nb][:, 0:512], hT3[:, c, :], wq3[:, c, nb * 512:(nb + 1) * 512], start=(c == 0), stop=(c == 7))
        for (dst3, b0) in ((qs3, 0), (ks3, 2)):
            for nb in range(2):
                src = pbs[b0 + nb].t.rearrange("p (h e) -> p h e", e=64)
                bb = pbs[b0 + nb].buf
                rope_apply(em, dst3[:, nb * 8:(nb + 1) * 8, 0:32], dst3[:, nb * 8:(nb + 1) * 8, 32:64],
                           V(src[:, :, 0:32], bb), V(src[:, :, 32:64], bb), bc_mid(cos, 8), bc_mid(sin, 8), ta, tb)
        v = vs[t % 2]
        em.copy('act', v[:, 0:512], pbs[4][:, 0:512])
        em.copy('act', v[:, 512:1024], pbs[5][:, 0:512])
        em.dma('sp', V(p.d_VA.t.rearrange("h t e -> t h e")[t * 128:(t + 1) * 128, :, :], p.d_VA.subbuf(t)),
               V(v.t.rearrange("p (h e) -> p h e", h=8), v.buf))
        qT = qTs[t % 2]
        kT = kTs[t % 2]
        for (src3, dstT, dd) in ((qs3, qT, QTd), (ks3, kT, KTd)):
            for hb in range(2):
                ptr = em.psum(6 + hb, BF16)
                for hh in range(8):
                    em.tr(ptr[0:64, hh * 128:(hh + 1) * 128], src3[:, hb * 8 + hh, :], p.ident[:, :])
                em.copy('act' if hb == 0 else 'dve', dstT[0:64, hb * 1024:(hb + 1) * 1024], ptr[0:64, :])
            em.dma('sp', V(dd[:, :, t * 128:(t + 1) * 128], p.d_QT.subbuf(t) if dd is QTd else p.d_KT.subbuf(t)),
                   V(dstT.t[0:64, :].rearrange("p (h t) -> p h t", h=16), dstT.buf))
    em.release(m1)
    m1 = em.mark()
    o0 = em.alloc("dfo0", NTOK)
    osb = em.alloc("dfosb", 512)
    rz = em.alloc("dfrz", 512)
    o1 = em.alloc("dfo1", 512)
    sq = em.alloc("dfsq", 512)
    rst = em.alloc("dfrst", 512)
    otn = [em.alloc("dfotn%d" % i, 512, BF16) for i in range(2)]
    fc = [0]

    def finish(hs, q0, nq, pO, pZ):
        h, s = divmod(hs, 2)
        em.copy('act', osb[:, 0:nq], pO[:, 0:nq])
        em.recip(rz[0:1, 0:nq], pZ[0:1, 0:nq])
        pb = em.psum(7)
        em.mm(pb[:, 0:nq], p.ones[0:1, 0:128], rz[0:1, 0:nq])
        if s == 0:
            em.tt('dve', o0[:, q0:q0 + nq], osb[:, 0:nq], pb[:, 0:nq], ALU.mult)
            return
        em.tt('dve', o1[:, 0:nq], osb[:, 0:nq], pb[:, 0:nq], ALU.mult)
        em.stt('dve', o1[:, 0:nq], o1[:, 0:nq], nlam, o0[:, q0:q0 + nq], ALU.mult, ALU.add)
        em.tt('pool', sq[:, 0:nq], o1[:, 0:nq], o1[:, 0:nq], ALU.mult)
        pb2 = em.psum(7)
        em.mm(pb2[:, 0:nq], p.ones[:, 0:128], sq[:, 0:nq])
        em.ts('dve', rst[:, 0:nq], pb2[:, 0:nq], 1.0 / 128, EPS, op0=ALU.mult, op1=ALU.add)
        em.act(rst[:, 0:nq], rst[:, 0:nq], AF.Ln)
        em.act(rst[:, 0:nq], rst[:, 0:nq], AF.Exp, scale=-0.5)
        i = fc[0] % 2
        fc[0] += 1
        on = otn[i]
        em.stt('dve', on[:, 0:nq], o1[:, 0:nq], subc[:, 1:2], rst[:, 0:nq], ALU.mult, ALU.mult)
        em.dma('sp', V(p.d_OT.t[h, :, q0:q0 + nq], p.d_OT.subbuf((h, q0))), on[:, 0:nq])

    class M:
        @staticmethod
        def vmap(h):
            return h // 2
    attention_core(em, p, M, 16, 64, 128, 64 ** -0.5, last, finish, zsep=True)
    em.release(m1)
    phase_oproj(em, p, p.d_diff_w_o, j, 8, 128, last)
    em.release(m0)


def cs(x, a, b):
    if isinstance(x, V):
        return V(x.ap[:, a:b], x.buf)
    return x[:, a:b]


def gla_scan_step(em, p, h4, qkT3, kr, vb, dcol, d, S32, Sbf, tri, po_banks, ps_att, ps_s, attm):
    pos = [em.psum(b) for b in po_banks]
    for h in range(4):
        pa = em.psum(ps_att)
        em.mm(pa[:, 0:128], qkT3[:, 4 + h, :], qkT3[:, h, :])
        am = attm[h % 2]
        em.tt('dve', am[:, 0:128], pa[:, 0:128], tri[:, d * 128:(d + 1) * 128], ALU.mult)
        po = pos[h // 2]
        oc = (h % 2) * 256
        em.mm(po[:, oc:oc + 256], am[:, 0:128], cs(vb, h * 256, (h + 1) * 256), start=True, stop=False)
        em.mm(po[:, oc:oc + 256], qkT3[:, h, :], Sbf[:, h * 256:(h + 1) * 256], start=False, stop=True)
        pS = em.psum(ps_s)
        em.mm(pS[:, 0:256], cs(kr, h * 128, (h + 1) * 128), cs(vb, h * 256, (h + 1) * 256))
        em.stt('dve', S32[:, h * 256:(h + 1) * 256], S32[:, h * 256:(h + 1) * 256], cs(dcol, d * 4 + h, d * 4 + h + 1),
               pS[:, 0:256], ALU.mult, ALU.add)
        em.copy('pool', Sbf[:, h * 256:(h + 1) * 256], S32[:, h * 256:(h + 1) * 256])
    return pos


def phase_gla(em, p, l, j, last):
    nc = em.nc
    NT, NC = p.NT, p.NC
    import os
    DBG = int(os.environ.get('GLA_DBG', '9'))
    m0 = em.mark()
    WIN = 3104
    win = em.alloc("glwin", 8 * WIN, BF16)
    win3 = r3(win, "p (c n) -> p c n", c=8)
    wv = p.d_gla_w_in.t[j].rearrange("(c p) n -> p c n", p=128)
    for i in range(4):
        n0, n1 = i * 776, (i + 1) * 776
        em.dma('pool', win3[:, :, n0:n1], V(wv[:, :, n0:n1], p.d_gla_w_in.buf))
    wgu = em.alloc("glwgu", 1024)
    for d in range(2):
        em.dma('sp', wgu[0:16, d * 512:(d + 1) * 512], V(p.d_gla_w_gate_up.t[j, d], p.d_gla_w_gate_up.buf))
    bg = em.alloc("glbg", 1024)
    em.dma('sp', bg[0:1, :], V(p.d_gla_b_gate.t[j:j + 1].rearrange("o d n -> o (d n)"), p.d_gla_b_gate.buf))
    tri = em.alloc("gltri", 256)
    for d in range(2):
        em.dma('sp', tri[:, d * 128:(d + 1) * 128], V(p.d_tri.t[d], p.d_tri.buf))
    hn = em.alloc("glhn", 256)
    em.dma('sp', hn[:, :], V(p.d_gla_head_norm.t[j].partition_broadcast(128), p.d_gla_head_norm.buf))
    S32 = em.alloc("glS32", 1024)
    Sbf = em.alloc("glSbf", 1024, BF16)
    attm = [em.alloc("glattm%d" % i, 128, BF16) for i in range(2)]
    gS = p.d_gS
    gO = p.d_gO
    if DBG <= 1:
        em.release(m0)
        return
    m1 = em.mark()
    ns = NormScratch(em, "gl")
    xts = [em.alloc("glxt%d" % i, D) for i in range(2)]
    hT = em.alloc("glhT", D, BF16)
    hT3 = r3(hT, "p (c t) -> p c t", c=8)
    q32 = em.alloc("glq32", 512)
    k32 = em.alloc("glk32", 512)
    z32 = em.alloc("glz32", 32)
    zT = em.alloc("glzT", 256)
    A = [em.alloc("gla%d" % d, 512) for d in range(2)]
    t1 = em.alloc("glt1", 512)
    t2 = em.alloc("glt2", 512)
    cums = em.alloc("glcum", 512)
    EE = em.alloc("glE", 512)
    qd = em.alloc("glqd", 512, BF16)
    ki = em.alloc("glki", 512, BF16)
    stg = [em.alloc("glstg%d" % i, 3584, BF16) for i in range(2)]
    qkTf = em.alloc("glqkTf", 1024, BF16)
    krf = em.alloc("glkrf", 512, BF16)
    ofs = [em.alloc("glof%d" % i, 1032) for i in range(2)]
    em.memset('dve', S32[:, :], 0.0)
    em.memset('dve', Sbf[:, :], 0.0)
    for t in range(NT):
        xt = xts[t % 2]
        load_x_tile(em, p, xt, t)
        r = 1 if t < NC else 0
        norm_tile(em, p, ns, xt[:, :], p.A1, p.B1, r, hT3[:, :, :], 7)
        pbs = [em.psum(i) for i in range(7)]
        for nb in range(6):
            for c in range(8):
                em.mm(pbs[nb][:, 0:512], hT3[:, c, :], win3[:, c, nb * 512:(nb + 1) * 512], start=(c == 0), stop=(c == 7))
        for c in range(8):
            em.mm(pbs[6][:, 0:32], hT3[:, c, :], win3[:, c, 3072:3104], start=(c == 0), stop=(c == 7))
        sg = stg[t % 2]
        em.ts('dve', q32[:, :], pbs[0][:, 0:512], 128 ** -0.5, None, op0=ALU.mult)
        em.copy('dve', k32[:, :], pbs[1][:, 0:512])
        em.copy('act', sg[:, 1536:2048], pbs[2][:, 0:512])
        em.copy('dve', sg[:, 2048:2560], pbs[3][:, 0:512])
        em.act(sg[:, 2560:3072], pbs[4][:, 0:512], AF.Silu)
        em.act(sg[:, 3072:3584], pbs[5][:, 0:512], AF.Silu)
        em.copy('dve', z32[:, :], pbs[6][:, 0:32])
        vb = sg[:, 1536:2560]
        if DBG <= 2:
            continue
        pz = em.psum(6)
        for d in range(2):
            em.mm(pz[0:16, d * 128:(d + 1) * 128], z32[:, d * 16:(d + 1) * 16], p.identf[:, :])
        em.copy('dve', zT[0:16, :], pz[0:16, 0:256])
        of = ofs[t % 2]
        pcol = em.psum(7)
        for d in range(2):
            pa = em.psum(d)
            em.mm(pa[:, 0:512], zT[0:16, d * 128:(d + 1) * 128], wgu[0:16, d * 512:(d + 1) * 512], start=True, stop=False)
            em.mm(pa[:, 0:512], p.ones[0:1, 0:128], bg[0:1, d * 512:(d + 1) * 512], start=False, stop=True)
            em.ts('dve', t2[:, :], pa[:, 0:512], 0.0, None, op0=ALU.min)
            em.stt('dve', t1[:, :], t2[:, :], -2.0, pa[:, 0:512], ALU.mult, ALU.add)
            em.act(t1[:, :], t1[:, :], AF.Exp, scale=-1.0)
            em.ts('dve', t1[:, :], t1[:, :], 1.0, None, op0=ALU.add)
            em.act(t1[:, :], t1[:, :], AF.Ln)
            em.tt('dve', t2[:, :], t2[:, :], t1[:, :], ALU.subtract)
            em.ts('dve', A[d][:, :], t2[:, :], 1.0 / 16, None, op0=ALU.mult)
            for h in range(4):
                em.mm(pcol[:, 2 * (d * 4 + h):2 * (d * 4 + h) + 2], A[d][:, h * 128:(h + 1) * 128], p.ones[:, 0:2])
        if DBG <= 3:
            continue
        em.act(t2[:, 0:16], pcol[:, 0:16], AF.Exp)
        em.copy('dve', of[:, 1024:1032], V(t2.t[:, 0:16].rearrange("p (a b) -> p a b", b=2)[:, :, 0], t2.buf))
        if DBG == 4 and os.environ.get('GLA_SUB') == 'a':
            continue
        dcol = of[:, 1024:1032]
        for d in range(2):
            pc = em.psum(2 + d)
            ptt = em.psum(4 + d)
            em.mm(pc[:, 0:512], tri[:, d * 128:(d + 1) * 128], A[d][:, :])
            em.mm(ptt[:, 0:512], p.ones[:, 0:128], A[d][:, :])
            em.copy('dve', cums[:, :], pc[:, 0:512])
            SUB = os.environ.get('GLA_SUB', 'z')
            if DBG == 4 and SUB == 'b':
                continue
            qdst = qd
            kidst = ki
            krdst = krf[:, :] if d == 0 else sg[:, 1024:1536]
            em.act(EE[:, :], cums[:, :], AF.Exp)
            em.tt('dve', qdst[:, :], q32[:, :], EE[:, :], ALU.mult)
            em.act(EE[:, :], cums[:, :], AF.Exp, scale=-1.0)
            em.tt('pool', kidst[:, :], k32[:, :], EE[:, :], ALU.mult)
            if DBG == 4 and SUB == 'c':
                continue
            em.tt('dve', t1[:, :], ptt[:, 0:512], cums[:, :], ALU.subtract)
            em.act(t1[:, :], t1[:, :], AF.Exp)
            em.tt('pool', krdst, k32[:, :], t1[:, :], ALU.mult)
            if DBG == 4 and SUB == 'd':
                continue
            ptr = em.psum(6, BF16)
            for h in range(4):
                em.tr(ptr[:, h * 128:(h + 1) * 128], qdst[:, h * 128:(h + 1) * 128], p.ident[:, :])
                em.tr(ptr[:, (4 + h) * 128:(5 + h) * 128], kidst[:, h * 128:(h + 1) * 128], p.ident[:, :])
            if d == 0:
                em.copy('act', qkTf[:, :], ptr[:, :])
            else:
                em.copy('act', sg[:, 0:1024], ptr[:, :])
        if DBG <= 4:
            continue
        qkT3 = r3(qkTf, "p (a t) -> p a t", a=8)
        pos = gla_scan_step(em, p, 4, qkT3, krf, vb, dcol, 0, S32, Sbf, tri, (0, 1), 2, 3, attm)
        em.copy('act', of[:, 0:512], pos[0][:, 0:512])
        em.copy('dve', of[:, 512:1024], pos[1][:, 0:512])
        em.dma('sp', V(gO.t[t], gO.subbuf(t)), of[:, :])
        em.dma('sp', V(gS.t[t], gS.subbuf(t)), sg[:, :])
    em.release(m1)
    if DBG <= 5:
        em.release(m0)
        return
    m1 = em.mark()
    wo = em.alloc("glwo", 8 * 1024, BF16)
    wo3 = r3(wo, "p (c n) -> p c n", c=8)
    em.dma('pool', wo3[:, :, :], V(p.d_gla_w_o.t[j].rearrange("(c p) n -> p c n", p=128), p.d_gla_w_o.buf))
    stg = [em.alloc("glstb%d" % i, 3584, BF16) for i in range(2)]
    ofs = [em.alloc("glofb%d" % i, 1032) for i in range(2)]
    xts = [em.alloc("glxb%d" % i, D) for i in range(2)]
    osum = em.alloc("glos", 1024)
    sq = em.alloc("glsq", 256)
    st = em.alloc("glst", 16)
    onb = em.alloc("glon", 1024, BF16)
    onT = em.alloc("glonT", 1024, BF16)
    onT3 = r3(onT, "p (c t) -> p c t", c=8)
    ys = [em.alloc("glys%d" % i, D) for i in range(2)]
    em.memset('dve', S32[:, :], 0.0)
    em.memset('dve', Sbf[:, :], 0.0)
    order = list(range(NC - 1, -1, -1)) + list(range(NT - 1, NC - 1, -1))
    for i, t in enumerate(order):
        sg = stg[i % 2]
        of = ofs[i % 2]
        xt = xts[i % 2]
        em.dma('sp', sg[:, :], V(gS.t[t], gS.subbuf(t)))
        em.dma('sp', of[:, :], V(gO.t[t], gO.subbuf(t)))
        load_x_tile(em, p, xt, t)
        qkT3 = r3(sg, "p (a t) -> p a t", a=28)
        pos = gla_scan_step(em, p, 4, qkT3, V(sg.t[:, 1024:1536], sg.buf), V(sg.t[:, 1536:2560], sg.buf), of[:, 1024:1032], 1,
                            S32, Sbf, tri, (0, 1), 2, 3, attm)
        for hb in range(2):
            em.tt('dve', osum[:, hb * 512:(hb + 1) * 512], pos[hb][:, 0:512], of[:, hb * 512:(hb + 1) * 512], ALU.add)
        for h in range(4):
            em.act(sq[:, :], osum[:, h * 256:(h + 1) * 256], AF.Square, accum=st[:, h:h + 1])
        rsqrt_col(em, st[:, 4:8], st[:, 0:4], 1.0 / 256, EPS, st[:, 8:12])
        for h in range(4):
            em.stt('dve', osum[:, h * 256:(h + 1) * 256], osum[:, h * 256:(h + 1) * 256], st[:, 4 + h:5 + h], hn[:, :], ALU.mult, ALU.mult)
        em.tt('pool', onb[:, :], osum[:, :], sg[:, 2560:3584], ALU.mult)
        ptr = em.psum(4, BF16)
        for c in range(8):
            em.tr(ptr[:, c * 128:(c + 1) * 128], onb[:, c * 128:(c + 1) * 128], p.ident[:, :])
        em.copy('act', onT[:, :], ptr[:, :])
        r = 1 if t < NC else 0
        y = ys[i % 2]
        for dh in range(2):
            py = em.psum(5 + dh)
            for c in range(8):
                em.mm(py[:, 0:512], onT3[:, c, :], wo3[:, c, dh * 512:(dh + 1) * 512], start=(c == 0), stop=(c == 7))
            em.tt('dve', y[:, dh * 512:(dh + 1) * 512], py[:, 0:512], p.G2[:, r * D + dh * 512:r * D + (dh + 1) * 512], ALU.mult)
        em.tt('pool', y[:, :], y[:, :], xt[:, :], ALU.add)
        em.dma('sp', xs_tile(p, t), y[:, :])
    em.release(m1)
    em.release(m0)


WNAMES = ['w_ada', 'b_ada', 'norm_mix', 'norm_ffn',
          'mla_w_dqkv', 'mla_q_norm', 'mla_w_uq', 'mla_kv_norm', 'mla_w_ukv', 'mla_w_o',
          'diff_w_qkv', 'diff_lambda', 'diff_subln', 'diff_w_o',
          'gla_w_in', 'gla_w_gate_up', 'gla_b_gate', 'gla_head_norm', 'gla_w_o',
          'moe_w_group', 'moe_b_group', 'moe_w_router', 'moe_b_router',
          'moe_w_gate', 'moe_w_up', 'moe_w_down', 'final_norm']


def declare_common(em, p, shapes, used):
    for nme in used:
        setattr(p, 'd_' + nme, em.dram(nme, shapes[nme], F32, kind="ExternalInput"))
    p.d_cc = em.dram("cc", [2, D], F32, kind="ExternalInput")
    p.d_ident = em.dram("ident", [128, 128], F32, kind="ExternalInput")
    p.d_sel2 = em.dram("sel2", [2, 256], F32, kind="ExternalInput")


def host_consts():
    sel2 = np.zeros((2, 256), np.float32)
    sel2[0, 0:128] = 1.0
    sel2[1, 128:256] = 1.0
    return dict(ident=np.eye(128, dtype=np.float32), sel2=sel2)


def core_tokens(x, ctx, b):
    return np.ascontiguousarray(np.concatenate([ctx[b], x[b]], axis=0))


def rope_table(S, nctx, rot_dim):
    n = np.arange(S)
    row = (n // 64).astype(np.float32)
    col = (n % 64).astype(np.float32)
    nf = rot_dim // 4
    inv = (10000.0 ** (-np.arange(nf, dtype=np.float32) / nf)).astype(np.float32)
    ang = np.concatenate([row[:, None] * inv, col[:, None] * inv], axis=-1).astype(np.float32)
    tab = np.concatenate([np.cos(ang), np.sin(ang)], axis=-1).astype(np.float32)
    half = rot_dim // 2
    ctab = np.concatenate([np.ones((nctx, half), np.float32), np.zeros((nctx, half), np.float32)], axis=-1)
    return np.ascontiguousarray(np.concatenate([ctab, tab], axis=0))


def declare_scratch_diff(em, p):
    NTOK = p.NT * 128
    p.d_QT = em.dram("dQT", [16, 64, NTOK], BF16)
    p.d_KT = em.dram("dKT", [16, 64, NTOK], BF16)
    p.d_VA = em.dram("dVA", [8, NTOK, 128], BF16)
    p.d_OT = em.dram("dOT", [8, 128, NTOK], BF16)


def declare_scratch_gla(em, p):
    p.d_gS = em.dram("gS", [p.NT, 128, 3584], BF16)
    p.d_gO = em.dram("gO", [p.NT, 128, 1032], F32)
    p.d_tri = em.dram("tri", [2, 128, 128], F32, kind="ExternalInput")


def use_scratch(p, d):
    for k, v in d.items():
        setattr(p, k, v)


def grab_scratch(p):
    return dict(d_QT=p.d_QT, d_KT=p.d_KT, d_VA=p.d_VA, d_OT=p.d_OT)


def host_tri():
    s = np.arange(128)
    U = (s[:, None] <= s[None, :]).astype(np.float32)
    L = (s[:, None] >= s[None, :]).astype(np.float32)
    return np.stack([U, L])


def declare_scratch_mla(em, p):
    NTOK = p.NT * 128
    p.d_QT = em.dram("sQT", [16, 96, NTOK], BF16)
    p.d_KT = em.dram("sKT", [16, 96, NTOK], BF16)
    p.d_VA = em.dram("sVA", [16, NTOK, 65], BF16)
    p.d_OT = em.dram("sOT", [16, 64, NTOK], BF16)


S_FULL = 8192
NCTX = 256
DEPTH = 4


def build_program(S, shapes):
    NC_ = NCTX // 128
    NT = NC_ + S // 128
    nc = bass.Bass("TRN2", target_bir_lowering=False)
    em = Em(nc)
    p = P()
    p.NT = NT
    p.NC = NC_
    declare_common(em, p, shapes, WNAMES)
    p.d_ropeA = em.dram("ropeA", [NT * 128, 32], F32, kind="ExternalInput")
    p.d_ropeB = em.dram("ropeB", [NT * 128, 64], F32, kind="ExternalInput")
    p.d_xin = em.dram("xin", [NT * 128, D], F32, kind="ExternalInput")
    p.d_xs = em.dram("xs", [NT * 128, D], F32)
    p.d_out = em.dram("out", [(NT - NC_) * 128, D], F32, kind="ExternalOutput")
    declare_scratch_mla(em, p)
    s_mla = grab_scratch(p)
    declare_scratch_diff(em, p)
    s_diff = grab_scratch(p)
    declare_scratch_gla(em, p)
    em.arena_init(47500)
    setup_consts(em, p)
    alloc_mod(em, p)
    for t in range(NT):
        em.dma('sp', xs_tile(p, t), V(p.d_xin.t[t * 128:(t + 1) * 128, :], p.d_xin.buf))
    for l in range(DEPTH):
        last = l == DEPTH - 1
        prologue(em, p, l)
        kind, j = l % 3, l // 3
        if kind == 0:
            use_scratch(p, s_mla)
            phase_mla(em, p, l, j, last)
        elif kind == 1:
            use_scratch(p, s_diff)
            phase_diff(em, p, l, j, last)
        else:
            phase_gla(em, p, l, j, last)
        phase_moe(em, p, l, last)
    phase_final(em, p)
    em.emit()
    return nc, em


def make_in_maps(inputs, S):
    x = np.asarray(inputs['x'], dtype=np.float32)
    ctx = np.asarray(inputs['ctx'], dtype=np.float32)
    B = x.shape[0]
    hc = host_consts()
    ropeA = rope_table(S, NCTX, 32)
    ropeB = rope_table(S, NCTX, 64)
    tri = host_tri()
    ws = {k: np.ascontiguousarray(np.asarray(inputs[k], dtype=np.float32)) for k in WNAMES}
    in_maps = []
    for b in range(B):
        m = dict(xin=core_tokens(x, ctx, b),
                 cc=np.ascontiguousarray(np.stack([np.asarray(inputs['c'], dtype=np.float32)[b], np.asarray(inputs['c_ctx'], dtype=np.float32)])),
                 ropeA=ropeA, ropeB=ropeB, tri=tri, **hc)
        m.update(ws)
        in_maps.append(m)
    return in_maps


def kernel(**inputs):
    x = np.asarray(inputs['x'])
    B, S, _ = x.shape
    shapes = {k: list(np.asarray(inputs[k]).shape) for k in WNAMES}
    nc, em = build_program(S, shapes)
    in_maps = make_in_maps(inputs, S)
    res = run_bass_kernel_spmd(nc, in_maps, core_ids=list(range(B)))
    out = np.stack([np.asarray(res.results[b]['out'], dtype=np.float32) for b in range(B)], axis=0)
    return out
```
